# Optimizing a Trainium2 kernel written in Bass

```python
import math
import jax
import jax.numpy as jnp
from jax import lax
import numpy as np

D_MODEL = 4096
BATCH = 2
SEQ = 8192
DEPTH = 2

GRID_W = 64
CTX_LEN = 256
N_BRANCH = 4
BRANCH_W = D_MODEL // 4
ATT_HEADS = 8
ATT_DH = BRANCH_W // (2 * ATT_HEADS)
ATT_DV = 2 * ATT_DH
ROPE_THETA = 10000.0
ROPE_FREQS = ATT_DH // 4
Q_BLOCK = 128
POOL_WINDOWS = (2, 4, 8, 16)
POOL_GROUPS = len(POOL_WINDOWS)
POOL_GW = BRANCH_W // POOL_GROUPS
FOURIER_GROUPS = 4
FOURIER_GW = BRANCH_W // FOURIER_GROUPS
CONV_K = 3
N_MOD = 6
D_FF = ((8 * D_MODEL // 3 + 255) // 256) * 256
N_EXPERTS = 8
TOP_K = 2
D_FF_EXPERT = D_FF // 4
N_DENSE = (DEPTH + 1) // 2
N_MOE = DEPTH // 2
EPS = 1e-6
Q_OFF = 0
K_OFF = BRANCH_W
V_OFF = 2 * BRANCH_W
POOL_OFF = 3 * BRANCH_W
FOUR_OFF = 4 * BRANCH_W
CB_OFF = 5 * BRANCH_W
CC_OFF = 6 * BRANCH_W
CH_OFF = 7 * BRANCH_W
IN_W = 8 * BRANCH_W

kernel_name = 'hybrid_parallel_mixer_moe_dit_block'


def rmsnorm(x, g):
    xf = x.astype(jnp.float32)
    y = xf * lax.rsqrt(jnp.mean(xf * xf, axis=-1, keepdims=True) + EPS)
    return (y * g.astype(jnp.float32)).astype(x.dtype)


def modulation(cond, w, b, n_chunks):
    cols = n_chunks * D_MODEL
    m = jax.nn.silu(cond) @ w[:, :cols] + b[:cols]
    m = m.reshape(-1, 1, cols)
    return jnp.split(m, n_chunks, axis=-1)


def modulate(xn, shift, scale):
    return xn * (1.0 + scale) + shift


def axial_rope_tables(rows):
    row = jnp.repeat(jnp.arange(rows, dtype=jnp.float32), GRID_W)
    col = jnp.tile(jnp.arange(GRID_W, dtype=jnp.float32), rows)
    freqs = ROPE_THETA ** (-jnp.arange(ROPE_FREQS, dtype=jnp.float32) / ROPE_FREQS)
    ang = jnp.stack([row[:, None] * freqs, col[:, None] * freqs], axis=1)
    return jnp.cos(ang), jnp.sin(ang)


def apply_axial_rope(x, cos, sin):
    shp = x.shape
    xs = x.reshape(shp[:-1] + (2, 2, ROPE_FREQS))
    x1, x2 = xs[..., 0, :], xs[..., 1, :]
    cs = cos[None, :, None, None].astype(x.dtype)
    sn = sin[None, :, None, None].astype(x.dtype)
    out = jnp.stack([x1 * cs - x2 * sn, x2 * cs + x1 * sn], axis=-2)
    return out.reshape(shp)


def split_q(pq, g):
    b, n, _ = pq.shape
    return rmsnorm(pq.reshape(b, n, ATT_HEADS, 2, ATT_DH), g)


def split_kv(pkv, g):
    b, n, _ = pkv.shape
    k = rmsnorm(pkv[..., :BRANCH_W].reshape(b, n, ATT_HEADS, 2, ATT_DH), g)
    v = pkv[..., BRANCH_W:].reshape(b, n, ATT_HEADS, ATT_DV)
    return k, v


def diff_softmax_attend(q, k, v, lam):
    s = jnp.einsum('bqhcd,bkhcd->bchqk', q, k, preferred_element_type=jnp.float32) * (ATT_DH ** -0.5)
    p = jax.nn.softmax(s, axis=-1)
    a = p[:, 0] - lam * p[:, 1]
    return jnp.einsum('bhqk,bkhe->bqhe', a, v)


def latent_diff_attention(q, k_all, v_all, lam):
    b, n = q.shape[0], q.shape[1]
    nb = n // Q_BLOCK
    qb = jnp.swapaxes(q.reshape(b, nb, Q_BLOCK, ATT_HEADS, 2, ATT_DH), 0, 1)
    ob = lax.map(lambda blk: diff_softmax_attend(blk, k_all, v_all, lam), qb)
    return jnp.swapaxes(ob, 0, 1).reshape(b, n, ATT_HEADS, ATT_DV)


def diff_head_out(o, g, lam_init, dtype):
    b, n = o.shape[0], o.shape[1]
    return (rmsnorm(o, g) * (1.0 - lam_init)).reshape(b, n, BRANCH_W).astype(dtype)


def multiscale_pool(u, pool_w, pool_scale):
    b, n, _ = u.shape
    ug = u.reshape(b, n, POOL_GROUPS, POOL_GW).astype(jnp.float32)
    csum = jnp.pad(jnp.cumsum(ug, axis=1), ((0, 0), (1, 0), (0, 0), (0, 0)))
    t = jnp.arange(n)
    outs = []
    for g, w in enumerate(POOL_WINDOWS):
        lo = jnp.clip(t - w // 2, 0, n - 1)
        hi = jnp.clip(t + w - 1 - w // 2, 0, n - 1)
        cnt = (hi - lo + 1).astype(jnp.float32)[None, :, None]
        cg = csum[:, :, g]
        win = (jnp.take(cg, hi + 1, axis=1) - jnp.take(cg, lo, axis=1)) / cnt
        outs.append(win - ug[:, :, g])
    pooled = jnp.stack(outs, axis=2).astype(u.dtype)
    y = jnp.einsum('bngc,gcd->bngd', pooled, pool_w)
    return y.reshape(b, n, BRANCH_W) * pool_scale


def fourier_mix(u):
    b, n, _ = u.shape
    ug = u.reshape(b, n, FOURIER_GROUPS, FOURIER_GW).astype(jnp.float32)
    y = jnp.fft.fft2(ug, axes=(1, 3), norm='ortho').real
    return y.reshape(b, n, BRANCH_W).astype(u.dtype)


def short_conv_mix(bg, cg, hv, conv_w):
    z = cg * hv
    n = z.shape[1]
    pad = CONV_K // 2
    zp = jnp.pad(z, ((0, 0), (pad, CONV_K - 1 - pad), (0, 0)))
    conv = None
    for j in range(CONV_K):
        term = conv_w[j] * zp[:, j:j + n]
        conv = term if conv is None else conv + term
    return bg * conv


def local_branches(p, pool_w, pool_scale, conv_w):
    pool = multiscale_pool(p[..., POOL_OFF:FOUR_OFF], pool_w, pool_scale)
    four = fourier_mix(p[..., FOUR_OFF:CB_OFF])
    conv = short_conv_mix(p[..., CB_OFF:CC_OFF], p[..., CC_OFF:CH_OFF], p[..., CH_OFF:IN_W], conv_w)
    return pool, four, conv


def merge_branches(xm, branches, w_gate, w_branch, w_out):
    gate_logits = xm @ w_gate
    merged = None
    for i, y in enumerate(branches):
        term = jax.nn.sigmoid(gate_logits[..., i * D_MODEL:(i + 1) * D_MODEL]) * (y @ w_branch[i])
        merged = term if merged is None else merged + term
    return merged @ w_out


def swiglu(z, w1, w3, w2):
    return (jax.nn.silu(z @ w1) * (z @ w3)) @ w2


def moe_swiglu(z, router, w1, w3, w2):
    logits = (z @ router).astype(jnp.float32)
    top_v, top_i = lax.top_k(logits, TOP_K)
    wts = jax.nn.softmax(top_v, axis=-1)
    combine = jnp.sum(jax.nn.one_hot(top_i, N_EXPERTS, dtype=jnp.float32) * wts[..., None], axis=-2).astype(z.dtype)
    out = None
    for e in range(N_EXPERTS):
        term = combine[..., e:e + 1] * swiglu(z, w1[e], w3[e], w2[e])
        out = term if out is None else out + term
    return out


def setup_inputs(seed: int = 0) -> dict:
    key = jax.random.key(seed)
    ks = jax.random.split(key, 32)
    D = D_MODEL

    def nrm(k, shape, s):
        return jax.random.normal(k, shape, jnp.float32) * s

    return {
        'x': nrm(ks[0], (BATCH, SEQ, D), 1.0),
        'c': nrm(ks[1], (BATCH, D), 1.0),
        'ctx': nrm(ks[2], (BATCH, CTX_LEN, D), 1.0),
        'c_ctx': nrm(ks[3], (D,), 1.0),
        'w_mod': nrm(ks[4], (DEPTH, D, N_MOD * D), 0.3 * D ** -0.5),
        'b_mod': nrm(ks[5], (DEPTH, N_MOD * D), 0.02),
        'norm_mix': 1.0 + nrm(ks[6], (DEPTH, D), 0.05),
        'norm_ffn': 1.0 + nrm(ks[7], (DEPTH, D), 0.05),
        'w_in': nrm(ks[8], (DEPTH, D, IN_W), D ** -0.5),
        'w_gate': nrm(ks[9], (DEPTH, D, N_BRANCH * D), D ** -0.5),
        'q_norm': 1.0 + nrm(ks[10], (DEPTH, ATT_DH), 0.05),
        'k_norm': 1.0 + nrm(ks[11], (DEPTH, ATT_DH), 0.05),
        'lambda_q1': nrm(ks[12], (DEPTH, ATT_DH), 0.1),
        'lambda_k1': nrm(ks[13], (DEPTH, ATT_DH), 0.1),
        'lambda_q2': nrm(ks[14], (DEPTH, ATT_DH), 0.1),
        'lambda_k2': nrm(ks[15], (DEPTH, ATT_DH), 0.1),
        'subln': 1.0 + nrm(ks[16], (DEPTH, ATT_DV), 0.05),
        'pool_w': nrm(ks[17], (DEPTH, POOL_GROUPS, POOL_GW, POOL_GW), POOL_GW ** -0.5),
        'pool_scale': 1.0 + nrm(ks[18], (DEPTH, BRANCH_W), 0.1),
        'conv_w': nrm(ks[19], (DEPTH, CONV_K, BRANCH_W), CONV_K ** -0.5),
        'w_branch': nrm(ks[20], (DEPTH, N_BRANCH, BRANCH_W, D), BRANCH_W ** -0.5),
        'w_out': nrm(ks[21], (DEPTH, D, D), D ** -0.5),
        'ffn_w1': nrm(ks[22], (N_DENSE, D, D_FF), D ** -0.5),
        'ffn_w3': nrm(ks[23], (N_DENSE, D, D_FF), D ** -0.5),
        'ffn_w2': nrm(ks[24], (N_DENSE, D_FF, D), D_FF ** -0.5),
        'router': nrm(ks[25], (N_MOE, D, N_EXPERTS), D ** -0.5),
        'moe_w1': nrm(ks[26], (N_MOE, N_EXPERTS, D, D_FF_EXPERT), D ** -0.5),
        'moe_w3': nrm(ks[27], (N_MOE, N_EXPERTS, D, D_FF_EXPERT), D ** -0.5),
        'moe_w2': nrm(ks[28], (N_MOE, N_EXPERTS, D_FF_EXPERT, D), D_FF_EXPERT ** -0.5),
    }


def reference(x, c, ctx, c_ctx, w_mod, b_mod, norm_mix, norm_ffn, w_in, w_gate, q_norm, k_norm,
              lambda_q1, lambda_k1, lambda_q2, lambda_k2, subln, pool_w, pool_scale, conv_w,
              w_branch, w_out, ffn_w1, ffn_w3, ffn_w2, router, moe_w1, moe_w3, moe_w2):
    n_lat = x.shape[1]
    rows = n_lat // GRID_W
    cos, sin = axial_rope_tables(rows)
    h, hc = x, ctx
    for l in range(DEPTH):
        last = l == DEPTH - 1
        lam_init = 0.8 - 0.6 * math.exp(-0.3 * l)
        lam = (jnp.exp(jnp.sum(lambda_q1[l].astype(jnp.float32) * lambda_k1[l].astype(jnp.float32)))
               - jnp.exp(jnp.sum(lambda_q2[l].astype(jnp.float32) * lambda_k2[l].astype(jnp.float32)))
               + lam_init)
        sh_m, sc_m, g_m, sh_f, sc_f, g_f = modulation(c, w_mod[l], b_mod[l], N_MOD)
        cm = modulation(c_ctx, w_mod[l], b_mod[l], 2 if last else N_MOD)

        xm = modulate(rmsnorm(h, norm_mix[l]), sh_m, sc_m)
        xcm = modulate(rmsnorm(hc, norm_mix[l]), cm[0], cm[1])

        if last:
            pc_kv = xcm @ w_in[l][:, K_OFF:POOL_OFF]
        else:
            pc = xcm @ w_in[l]
            pc_kv = pc[..., K_OFF:POOL_OFF]
        k_c, v_c = split_kv(pc_kv, k_norm[l])

        p = xm @ w_in[l]
        q = apply_axial_rope(split_q(p[..., Q_OFF:K_OFF], q_norm[l]), cos, sin)
        k, v = split_kv(p[..., K_OFF:POOL_OFF], k_norm[l])
        k = apply_axial_rope(k, cos, sin)
        k_all = jnp.concatenate([k_c, k], axis=1)
        v_all = jnp.concatenate([v_c, v], axis=1).astype(jnp.float32)
        att = diff_head_out(latent_diff_attention(q, k_all, v_all, lam), subln[l], lam_init, x.dtype)
        branches = (att,) + local_branches(p, pool_w[l], pool_scale[l], conv_w[l])
        h = h + g_m * merge_branches(xm, branches, w_gate[l], w_branch[l], w_out[l])

        if not last:
            q_c = split_q(pc[..., Q_OFF:K_OFF], q_norm[l])
            att_c = diff_head_out(diff_softmax_attend(q_c, k_c, v_c.astype(jnp.float32), lam),
                                  subln[l], lam_init, x.dtype)
            branches_c = (att_c,) + local_branches(pc, pool_w[l], pool_scale[l], conv_w[l])
            hc = hc + cm[2] * merge_branches(xcm, branches_c, w_gate[l], w_branch[l], w_out[l])

        def channel_mix(z):
            if l % 2 == 0:
                return swiglu(z, ffn_w1[l // 2], ffn_w3[l // 2], ffn_w2[l // 2])
            return moe_swiglu(z, router[l // 2], moe_w1[l // 2], moe_w3[l // 2], moe_w2[l // 2])

        h = h + g_f * channel_mix(modulate(rmsnorm(h, norm_ffn[l]), sh_f, sc_f))
        if not last:
            hc = hc + cm[5] * channel_mix(modulate(rmsnorm(hc, norm_ffn[l]), cm[3], cm[4]))
    return h
```

```python
import math
from contextlib import ExitStack
import numpy as np
import ml_dtypes
import concourse.bass as bass
import concourse.mybir as mybir
from concourse.bass_utils import run_bass_kernel_spmd

F32 = mybir.dt.float32
BF16 = mybir.dt.bfloat16
I32 = mybir.dt.int32
ALU = mybir.AluOpType
AF = mybir.ActivationFunctionType
AX = mybir.AxisListType
EPS = 1e-6


class Cfg:
    def __init__(s, D=4096, SEQ=8192, CTX=256, GRID_W=64, H=8, POOLW=(2, 4, 8, 16), FG=4, NE=8,
                 T1=256, T3=256, DEPTH=2):
        s.D, s.SEQ, s.CTX, s.GRID_W, s.H, s.POOLW, s.FG, s.NE = D, SEQ, CTX, GRID_W, H, tuple(POOLW), FG, NE
        s.DEPTH = DEPTH
        s.B = 2
        s.NCORES = 8
        s.CPB = 4
        s.BW = D // 4
        s.DH = s.BW // (2 * H)
        assert 2 * s.DH == 128
        s.HC = s.BW // 128
        s.INW = 8 * s.BW
        s.PG = len(POOLW)
        s.PGW = s.BW // s.PG
        s.PKC = s.PGW // 128
        s.FGW = s.BW // FG
        s.FKC = s.FGW // 128
        s.DFF = ((8 * D // 3 + 255) // 256) * 256
        s.DFE = s.DFF // 4
        s.NL = SEQ // s.CPB
        s.NC = CTX // s.CPB
        s.NTOK = s.NL + s.NC
        s.DC = D // 128
        s.MODC = 6 * D // 8
        s.MC = s.MODC // 128
        s.T1 = min(T1, s.NL)
        s.T3 = min(T3, s.NL)
        s.RF = s.DH // 4
        s.NWB = 3
        o = 0
        s.V = {}
        for name, n in (("norm_mix", s.DC), ("norm_ffn", s.DC), ("b_mod", 6 * s.DC), ("q_norm", 1), ("k_norm", 1),
                        ("subln", 1), ("pool_scale", s.HC), ("conv_w", 3 * s.HC)):
            s.V[name] = (o, n)
            o += n
        s.NV = o


FULL = Cfg()


class _Stop(Exception):
    pass


class Sched:
    LIMIT = 30000

    def __init__(self, nc, ndma=40):
        self.nc = nc
        self.eng = {"pe": nc.tensor, "act": nc.scalar, "dve": nc.vector, "pool": nc.gpsimd, "sp": nc.sync}
        self.sems = {}
        self.cnt = {}
        self.cur = {}
        self.gen = {}
        for e in self.eng:
            self.gen[e] = 0
            self._new_sem(e)
        self.dma_names = []
        for i in range(ndma):
            n = "dma%d" % i
            self.sems[n] = nc.alloc_semaphore(n)
            self.cnt[n] = 0
            self.dma_names.append(n)
        self.sems["cc"] = nc.alloc_semaphore("ccsem")
        self.cnt["cc"] = 0
        self.dma_rr = 0
        self.waited = {e: {} for e in self.eng}
        self.last_w = {}
        self.readers = {}
        self.n_instr = 0

    def _new_sem(self, e):
        n = "sem_%s_%d" % (e, self.gen[e])
        self.gen[e] += 1
        self.sems[n] = self.nc.alloc_semaphore(n)
        self.cnt[n] = 0
        self.cur[e] = n

    def _deps(self, e, r, w):
        deps = {}

        def add(ev):
            if ev is None:
                return
            s, v = ev
            if deps.get(s, 0) < v:
                deps[s] = v

        for k in r:
            add(self.last_w.get(k))
        for k in w:
            add(self.last_w.get(k))
            for ev in self.readers.get(k, ()):
                add(ev)
        for s, v in deps.items():
            if e == "pe" and s.startswith("sem_pe_"):
                continue
            if self.waited[e].get(s, 0) < v:
                self.eng[e].wait_ge(self.sems[s], v)
                self.waited[e][s] = v
                self.n_instr += 1

    def _record(self, ev, r, w):
        for k in r:
            self.readers.setdefault(k, []).append(ev)
        for k in w:
            self.last_w[k] = ev
            self.readers[k] = []

    def op(self, e, fn, r=(), w=()):
        self._deps(e, r, w)
        ins = fn()
        if self.cnt[self.cur[e]] >= self.LIMIT:
            self._new_sem(e)
        n = self.cur[e]
        self.cnt[n] += 1
        ins.then_inc(self.sems[n], 1)
        self.n_instr += 1
        self._record((n, self.cnt[n]), r, w)

    def dma(self, q, out, in_, r=(), w=()):
        self._deps(q, r, w)
        n = self.dma_names[self.dma_rr]
        self.dma_rr = (self.dma_rr + 1) % len(self.dma_names)
        if self.waited[q].get(n, 0) < self.cnt[n]:
            self.eng[q].wait_ge(self.sems[n], self.cnt[n])
            self.waited[q][n] = self.cnt[n]
        self.eng[q].dma_start(out=out, in_=in_).then_inc(self.sems[n], 16)
        self.cnt[n] += 16
        self.n_instr += 1
        self._record((n, self.cnt[n]), r, w)

    def gather(self, groups, src, dst, r=(), w=()):
        q = "pool"
        self._deps(q, r, w)
        self.nc.gpsimd.collective_compute("AllGather", ALU.bypass, replica_groups=groups, ins=[src], outs=[dst]
                                          ).then_inc(self.sems["cc"])
        self.cnt["cc"] += 1
        self.n_instr += 1
        self._record(("cc", self.cnt["cc"]), r, w)

    def barrier(self):
        for e in self.eng:
            for s, v in self.cnt.items():
                if v > 0 and self.waited[e].get(s, 0) < v and not s.startswith("sem_%s_" % e):
                    self.eng[e].wait_ge(self.sems[s], v)
                    self.waited[e][s] = v
        self.last_w = {}
        self.readers = {}

    def final_wait(self, e="pool"):
        for s, v in self.cnt.items():
            if v > 0 and not s.startswith("sem_%s_" % e) and self.waited[e].get(s, 0) < v:
                self.eng[e].wait_ge(self.sems[s], v)
                self.waited[e][s] = v


def psk(ap):
    return ("ps", ap.tensor.name)


def piece_rows(K, N):
    best = 8
    for M in range(8, K + 1, 8):
        if K % M == 0 and M * N * 2 <= 8 * 2 ** 20:
            best = M
    return best


def tok_pieces(NL, NC, m):
    out = [(t0, min(m, NL - t0)) for t0 in range(0, NL, m)]
    if NC > 0:
        out.append((NL, NC))
    return out


def piece_row(pcs, r, o):
    for (t0, m) in pcs:
        if t0 <= o < t0 + m:
            return 4 * t0 + r * m + (o - t0)
    raise AssertionError


def chunks(n, c=128):
    out = []
    o = 0
    while o < n:
        out.append((o, min(c, n - o)))
        o += c
    return out


def build_program(cfg):
    g = cfg
    nc = bass.Bass("TRN2", target_bir_lowering=False)
    S = Sched(nc)
    D, DC, BW, HC, NL, NC_, NTOK, L = g.D, g.DC, g.BW, g.HC, g.NL, g.NC, g.NTOK, g.DEPTH
    GRP4 = [[0, 1, 2, 3], [4, 5, 6, 7]]
    vpcs = tok_pieces(NL, NC_, max(128, (262144 // BW) // 128 * 128))
    gpcs = tok_pieces(NL, NC_, max(128, (262144 // (2 * BW)) // 128 * 128))
    GRP8 = [list(range(8))]

    uid = [0]

    def un(name):
        uid[0] += 1
        return "%s_u%d" % (name, uid[0])

    def din(name, shape, dt=F32):
        return nc.dram_tensor(name, list(shape), dt, kind="ExternalInput").ap()

    def dint(name, shape, dt):
        return nc.dram_tensor(name, list(shape), dt).ap()

    xT = din("xT", [D, NTOK])
    condT = din("condT", [128, DC * 3])
    wmod = din("wmod", [L, D, g.MODC])
    vecs = din("vecs", [128, L * g.NV])
    lamv = din("lamv", [128, L * 4 * g.DH])
    routerT = din("routerT", [128, DC * g.NE])
    ropec = din("ropec", [128, NTOK])
    ropes = din("ropes", [128, NTOK])
    kpos = din("kpos", [1, NTOK])
    invcnt = din("invcnt", [g.PG, NTOK])
    misc = din("misc", [128, 16])
    cmat = din("cmat", [128, 6 * 128])
    csmat = din("csmat", [g.FGW, 2 * g.FGW], BF16)
    eselm = din("eselm", [128, g.NE * 128])
    outT = nc.dram_tensor("outT", [D, NL], F32, kind="ExternalOutput").ap()

    wspecs = {
        "w_in": (D, g.INW, L), "w_gate": (D, 4 * D, L), "w_branch": (4 * BW, D, L), "w_out": (D, D, L),
        "pool_w": (g.PG * g.PGW, g.PGW, L),
        "ffn_w1": (D, g.DFF, 1), "ffn_w3": (D, g.DFF, 1), "ffn_w2": (g.DFF, D, 1),
        "moe_w1": (g.NE * D, g.DFE, 1), "moe_w3": (g.NE * D, g.DFE, 1), "moe_w2": (g.NE * g.DFE, D, 1),
    }
    wsrc, wcast, wfull = {}, {}, {}
    for name, (K, N, nl) in wspecs.items():
        F = (K // 8) * N // 128
        assert (K // 8) * N % 128 == 0
        for l in range(nl):
            wsrc[(name, l)] = din("%s_%d" % (name, l), [128, F])
            wcast[(name, l)] = dint("%s_c%d" % (name, l), [128, F], BF16)
            wfull[(name, l)] = dint("%s_f%d" % (name, l), [K, N], BF16)

    def wview(name, l):
        K, N, nl = wspecs[name]
        return wfull[(name, l)]

    hA = dint("hA", [D, NTOK], F32)
    hmid = dint("hmid", [D, NTOK], F32)
    hmid2 = dint("hmid2", [D, NTOK], F32)
    xmT = dint("xmT", [D, NTOK], BF16)
    qT = dint("qT", [BW, NTOK], BF16)
    kT = dint("kT", [BW, NTOK], BF16)
    kG = dint("kG", [4 * BW, NTOK], BF16)
    vS = dint("vS", [NTOK, BW], BF16)
    vG = dint("vG", [4 * NTOK, BW], BF16)
    gS = dint("gS", [NTOK, 2 * BW], BF16)
    gG = dint("gG", [4 * NTOK, 2 * BW], BF16)
    upool = dint("upool", [BW, NTOK], F32)
    zT = dint("zT", [BW, NTOK], F32)
    cbT = dint("cbT", [BW, NTOK], F32)
    slab = dint("slab", [2 * BW, 32], F32)
    slabG = dint("slabG", [4 * 2 * BW, 32], F32)
    yT = dint("yT", [4 * BW, NTOK], BF16)
    modS = dint("modS", [128, g.MC * 3], F32)
    modG = dint("modG", [8 * 128, g.MC * 3], F32)
    NLB = NL * g.CPB
    NCB = NC_ * g.CPB
    twL = dint("twL", [2 * (NLB // 128) * 128, NL], BF16)
    twC = dint("twC", [2 * g.CPB * 128, max(NC_, 1)], BF16)

    def sb(name, shape, dt=F32):
        return nc.alloc_sbuf_tensor(name, list(shape), dt).ap()

    cm = sb("cm", [128, 6 * 128])
    ident, Rm, onesD, blk64, ones128, eselb = [cm[:, i * 128:(i + 1) * 128] for i in range(6)]
    onesb = sb("onesb", [128, 128], BF16)
    vsb = sb("vsb", [128, L * g.NV])
    msk = sb("msk", [128, 16])
    scond = sb("scond", [128, DC * 3])
    modl = sb("modl", [128, 6 * DC])
    modc = sb("modc", [128, 6 * DC])
    Amix = sb("Amix", [128, 2 * DC])
    Affn = sb("Affn", [128, 2 * DC])
    lam = sb("lam", [128, 4])
    sgv = sb("sgv", [128, 1])
    rt = sb("rt", [128, DC * g.NE])
    esel = sb("esel", [128, g.NE, 128])
    ps = [nc.alloc_psum_tensor("ps%d" % i, [128, 512], F32).ap() for i in range(8)]

    def rsq(out, inp, rk, wk):
        S.op("dve", lambda: nc.vector.tensor_scalar(out=out, in0=inp, scalar1=EPS, scalar2=None, op0=ALU.add), r=rk, w=wk)
        S.op("act", lambda: nc.scalar.activation(out=out, in_=out, func=AF.Sqrt), r=wk, w=wk)
        S.op("dve", lambda: nc.vector.reciprocal(out=out, in_=out), r=wk, w=wk)

    S.dma("sp", cm, cmat, w=["cm"])
    S.dma("sp", vsb, vecs, w=["vsb"])
    S.dma("sp", msk, misc, w=["msk"])
    S.dma("sp", scond, condT, w=["scond"])
    S.dma("sp", rt, routerT, w=["rt"])
    S.dma("sp", esel, eselm.rearrange("p (e m) -> p e m", e=g.NE), w=["esel"])
    S.op("dve", lambda: nc.vector.memset(onesb, 1.0), w=["onesb"])
    S.op("act", lambda: nc.scalar.activation(out=scond, in_=scond, func=AF.Silu), r=["scond"], w=["scond"])

    def vec(l, name, i=0, n=1):
        o, _ = g.V[name]
        return vsb[:, l * g.NV + o + i: l * g.NV + o + i + n]

    with ExitStack() as es:
        CP = 8192
        cin = [es.enter_context(nc.sbuf_tensor(un("cin%d" % i), [128, CP], F32)).ap() for i in range(2)]
        cout = [es.enter_context(nc.sbuf_tensor(un("cout%d" % i), [128, CP], BF16)).ap() for i in range(2)]
        it = 0
        for (name, l), src in wsrc.items():
            F = src.shape[1]
            for (o, n) in chunks(F, CP):
                b = it % 2
                S.dma("sp", cin[b][:, :n], src[:, o:o + n], w=[("cin", b)])
                eng = "dve" if it % 2 == 0 else "pool"
                if eng == "dve":
                    S.op("dve", lambda b=b, n=n: nc.vector.tensor_copy(out=cout[b][:, :n], in_=cin[b][:, :n]),
                         r=[("cin", b)], w=[("cout", b)])
                else:
                    S.op("act", lambda b=b, n=n: nc.scalar.copy(out=cout[b][:, :n], in_=cin[b][:, :n]),
                         r=[("cin", b)], w=[("cout", b)])
                S.dma("pool", wcast[(name, l)][:, o:o + n], cout[b][:, :n], r=[("cout", b)], w=[("wc", name, l)])
                it += 1
            K_, N_, _ = wspecs[name]
            M_ = piece_rows(K_, N_)
            E_ = (M_ // 8) * N_
            flat = wcast[(name, l)].rearrange("p f -> (p f)")
            for j in range(K_ // M_):
                S.gather(GRP8, flat[j * E_:(j + 1) * E_].rearrange("(r n) -> r n", n=N_),
                         wfull[(name, l)][j * M_:(j + 1) * M_, :], r=[("wc", name, l)], w=[("wf", name, l, j)])

        kv = es.enter_context(nc.sbuf_tensor(un("kv"), [128, NTOK], F32)).ap()
        pv = es.enter_context(nc.sbuf_tensor(un("pvv"), [128, 1], F32)).ap()
        S.dma("sp", kv, kpos.partition_broadcast(128), w=["kv"])
        S.op("pool", lambda: nc.gpsimd.iota(pv, pattern=[[0, 1]], base=0, channel_multiplier=1,
                                            allow_small_or_imprecise_dtypes=True), w=["pv"])
        TWW = min(512, NL)
        tA = es.enter_context(nc.sbuf_tensor(un("tA"), [128, TWW], F32)).ap()
        tB = es.enter_context(nc.sbuf_tensor(un("tB"), [128, TWW], F32)).ap()
        tI = es.enter_context(nc.sbuf_tensor(un("tI"), [128, TWW], I32)).ap()
        tO = [es.enter_context(nc.sbuf_tensor(un("tO%d" % i), [128, TWW], BF16)).ap() for i in range(2)]
        negpi = es.enter_context(nc.sbuf_tensor(un("negpi"), [128, 1], F32)).ap()
        S.op("dve", lambda: nc.vector.memset(negpi, -math.pi), w=["negpi"])

        def gen_tw(dst, nchunks, cs, N, k0, kn, kcol0):
            oi = 0
            for a in range(nchunks):
                for (ko, kw) in chunks(kn, TWW):
                    kk = kv[:, kcol0 + ko: kcol0 + ko + kw]
                    A_, B_, I_ = tA[:, :kw], tB[:, :kw], tI[:, :kw]
                    S.op("dve", lambda: nc.vector.tensor_scalar(out=A_, in0=kk, scalar1=float(a), scalar2=None,
                                                                op0=ALU.mult), r=["kv"], w=["tA"])
                    S.op("dve", lambda: nc.vector.tensor_copy(out=I_, in_=A_), r=["tA"], w=["tI"])
                    S.op("dve", lambda: nc.vector.tensor_single_scalar(out=I_, in_=I_, scalar=N // cs - 1,
                                                                       op=ALU.bitwise_and), r=["tI"], w=["tI"])
                    S.op("dve", lambda: nc.vector.tensor_copy(out=A_, in_=I_), r=["tI"], w=["tA"])
                    S.op("dve", lambda: nc.vector.tensor_scalar(out=A_, in0=A_, scalar1=float(cs), scalar2=None,
                                                                op0=ALU.mult), r=["tA"], w=["tA"])
                    S.op("dve", lambda: nc.vector.scalar_tensor_tensor(out=B_, in0=kk, scalar=pv[:, 0:1], in1=A_,
                                                                       op0=ALU.mult, op1=ALU.add),
                         r=["kv", "pv", "tA"], w=["tB"])
                    for ci in range(2):
                        sh = float(N // 4) if ci == 0 else 0.0
                        S.op("dve", lambda: nc.vector.tensor_scalar(out=A_, in0=B_, scalar1=sh, scalar2=None,
                                                                    op0=ALU.add), r=["tB"], w=["tA"])
                        S.op("dve", lambda: nc.vector.tensor_copy(out=I_, in_=A_), r=["tA"], w=["tI"])
                        S.op("dve", lambda: nc.vector.tensor_single_scalar(out=I_, in_=I_, scalar=N - 1,
                                                                           op=ALU.bitwise_and), r=["tI"], w=["tI"])
                        S.op("dve", lambda: nc.vector.tensor_copy(out=A_, in_=I_), r=["tI"], w=["tA"])
                        ob = tO[oi % 2]
                        oi += 1
                        S.op("act", lambda ob=ob: nc.scalar.activation(out=ob[:, :kw], in_=A_, func=AF.Sin,
                                                                       bias=negpi[:, 0:1], scale=2.0 * math.pi / N),
                             r=["tA", "negpi"], w=[("tO", id(ob))])
                        r0 = ci * nchunks * 128 + a * 128
                        S.dma("pool", dst[r0:r0 + 128, ko:ko + kw], ob[:, :kw], r=[("tO", id(ob))], w=["tw"])

        gen_tw(twL, NLB // 128, 128, NLB, 0, NL, 0)
        gen_tw(twC, g.CPB, NC_, NCB, 0, NC_, NL)
    S.barrier()

    WB_K, WB_N = 22, 512

    def gemm(W, col0, ncols, kgroups, T, epi, wbufs, banks, tm=False, tag="g"):
        colch = chunks(ncols, 128)
        tokch = chunks(T, 128)
        nk = sum(len(kg) for kg in kgroups)
        ki = 0
        state = gemm.state

        def load(kg):
            b = state["wb"] % len(wbufs)
            state["wb"] += 1
            wb = wbufs[b]
            i = 0
            while i < len(kg):
                r0, nr, _ = kg[i]
                if nr == 128:
                    j = i
                    while j + 1 < len(kg) and kg[j + 1][1] == 128 and kg[j + 1][0] == kg[j][0] + 128:
                        j += 1
                    n = j - i + 1
                    for (so, sn) in chunks(n, 8):
                        S.dma("sp", wb[:, i + so:i + so + sn, :ncols],
                              W[r0 + so * 128: r0 + (so + sn) * 128, col0:col0 + ncols].rearrange(
                                  "(c p) n -> p c n", p=128),
                              r=[("wf", W.tensor.name)], w=[("wb", b)])
                    i = j + 1
                else:
                    S.dma("sp", wb[:nr, i, :ncols], W[r0:r0 + nr, col0:col0 + ncols],
                          r=[("wf", W.tensor.name)], w=[("wb", b)])
                    i += 1
            return b

        nxt = load(kgroups[0])
        for gi, kg in enumerate(kgroups):
            b = nxt
            if gi + 1 < len(kgroups):
                nxt = load(kgroups[gi + 1])
            wb = wbufs[b]
            for i, (r0, nr, act) in enumerate(kg):
                first, last = (ki == 0), (ki == nk - 1)
                if not tm:
                    for j, (co, cw) in enumerate(colch):
                        S.op("pe", lambda j=j, co=co, cw=cw: nc.tensor.matmul(
                            banks[j][:cw, :T], lhsT=wb[:nr, i, co:co + cw], rhs=act, start=first, stop=last),
                            r=[("wb", b), act_key(act)], w=[psk(banks[j])])
                else:
                    for j, (to, tw_) in enumerate(tokch):
                        S.op("pe", lambda j=j, to=to, tw_=tw_: nc.tensor.matmul(
                            banks[j][:tw_, :ncols], lhsT=act[:, to:to + tw_], rhs=wb[:nr, i, :ncols],
                            start=first, stop=last),
                            r=[("wb", b), act_key(act)], w=[psk(banks[j])])
                ki += 1
        if not tm:
            for j, (co, cw) in enumerate(colch):
                epi(j, col0 + co, cw, banks[j][:cw, :T])
        else:
            for j, (to, tw_) in enumerate(tokch):
                epi(j, to, tw_, banks[j][:tw_, :ncols])

    gemm.state = {"wb": 0}
    akeys = {}

    def act_key(ap):
        return akeys.get(id(ap), ("act", id(ap)))

    def reg(ap, key):
        akeys[id(ap)] = key
        return ap

    def kg_full(K, acts, per=16):
        ents = []
        for ci, (o, n) in enumerate(chunks(K, 128)):
            ents.append((o, n, acts[ci]))
        return [ents[i:i + per] for i in range(0, len(ents), per)]

    def norm_mod(src, t0, T, A, Bv, out_sb, okey, bufs, router=None):
        hb, sq, rstd, tmp = bufs
        pstat = ps[7]
        for c in range(DC):
            h_ = hb[c % 4]
            S.dma("sp", h_[:, :T], src[c * 128:(c + 1) * 128, t0:t0 + T], r=[("h", src.tensor.name)], w=[("hb", c % 4)])
            s_ = sq[c % 2]
            S.op("act", lambda h_=h_, s_=s_: nc.scalar.activation(out=s_[:, :T], in_=h_[:, :T], func=AF.Square),
                 r=[("hb", c % 4)], w=[("sq", c % 2)])
            S.op("pe", lambda s_=s_, c=c: nc.tensor.matmul(pstat[:, :T], lhsT=onesD, rhs=s_[:, :T], start=(c == 0),
                                                          stop=(c == DC - 1)), r=[("sq", c % 2), "cm"], w=[psk(pstat)])
        rsq(rstd[:, :T], pstat[:, :T], [psk(pstat)], ["rstd"])
        for c in range(DC):
            h_ = hb[c % 4]
            S.dma("sp", h_[:, :T], src[c * 128:(c + 1) * 128, t0:t0 + T], r=[("h", src.tensor.name)], w=[("hb", c % 4)])
            t_ = tmp[c % 2]
            S.op("dve", lambda h_=h_, t_=t_, c=c: nc.vector.scalar_tensor_tensor(
                out=t_[:, :T], in0=h_[:, :T], scalar=A[:, c:c + 1], in1=rstd[:, :T], op0=ALU.mult, op1=ALU.mult),
                r=[("hb", c % 4), "rstd", "mod"], w=[("tmp", c % 2)])
            if router is None:
                S.op("act", lambda t_=t_, c=c: nc.scalar.activation(out=out_sb[:, c, :T], in_=t_[:, :T], func=AF.Identity,
                                                                    bias=Bv[:, c:c + 1], scale=1.0),
                     r=[("tmp", c % 2), "mod"], w=[okey])
            else:
                zf = router["zf"][c % 2]
                S.op("act", lambda t_=t_, c=c, zf=zf: nc.scalar.activation(out=zf[:, :T], in_=t_[:, :T], func=AF.Identity,
                                                                           bias=Bv[:, c:c + 1], scale=1.0),
                     r=[("tmp", c % 2), "mod"], w=[("zf", c % 2)])
                S.op("dve", lambda c=c, zf=zf: nc.vector.tensor_copy(out=out_sb[:, c, :T], in_=zf[:, :T]),
                     r=[("zf", c % 2)], w=[okey])
                for si, (to, tw_) in enumerate(chunks(T, 128)):
                    S.op("pe", lambda c=c, zf=zf, si=si, to=to, tw_=tw_: nc.tensor.matmul(
                        ps[5 + si][:tw_, :g.NE], lhsT=zf[:, to:to + tw_], rhs=rt[:, c * g.NE:(c + 1) * g.NE],
                        start=(c == 0), stop=(c == DC - 1)), r=[("zf", c % 2), "rt"], w=[psk(ps[5 + si])])

    stop = getattr(g, 'STOP', None)
    try:
        if stop == 'A':
            raise _Stop()
        tiles_lat = [(o, n, False) for (o, n) in chunks(NL, g.T1)]
        tile_ctx = (NL, NC_, True)

        for l in range(L):
            last = (l == L - 1)
            lam_init = 0.8 - 0.6 * math.exp(-0.3 * l)
            h_src = xT if l == 0 else hA
            h_dst = outT if last else hA
            moe = (l % 2 == 1)

            with ExitStack() as es:
                wm = [es.enter_context(nc.sbuf_tensor(un("wm%d" % i), [128, DC, 128], F32)).ap() for i in range(2)]
                mloc = es.enter_context(nc.sbuf_tensor(un("mloc"), [128, g.MC * 3], F32)).ap()
                mg = es.enter_context(nc.sbuf_tensor(un("mg"), [128, 8 * g.MC * 3], F32)).ap()
                lv = es.enter_context(nc.sbuf_tensor(un("lv"), [128, 4 * g.DH], F32)).ap()
                lt = es.enter_context(nc.sbuf_tensor(un("lt"), [128, g.DH], F32)).ap()
                ls = es.enter_context(nc.sbuf_tensor(un("ls"), [128, 2], F32)).ap()
                pm = ps[0]
                for j in range(g.MC):
                    b = j % 2
                    for (so, sn) in chunks(DC, 8):
                        S.dma("sp", wm[b][:, so:so + sn, :],
                              wmod[l, so * 128:(so + sn) * 128, j * 128:(j + 1) * 128].rearrange("(c p) n -> p c n", p=128),
                              w=[("wm", b)])
                    for c in range(DC):
                        S.op("pe", lambda b=b, c=c, j=j: nc.tensor.matmul(
                            pm[:, 3 * j:3 * j + 3], lhsT=wm[b][:, c, :], rhs=scond[:, c * 3:c * 3 + 3],
                            start=(c == 0), stop=(c == DC - 1)), r=[("wm", b), "scond"], w=[psk(pm)])
                S.op("dve", lambda: nc.vector.tensor_copy(out=mloc, in_=pm[:, :g.MC * 3]), r=[psk(pm)], w=["mloc"])
                S.dma("pool", modS, mloc, r=["mloc"], w=["modS"])
                S.gather(GRP8, modS, modG, r=["modS"], w=["modG"])
                S.dma("sp", mg.rearrange("p (r m) -> p r m", r=8), modG.rearrange("(r p) m -> p r m", p=128),
                      r=["modG"], w=["mg"])
                mg3 = mg.rearrange("p (k j) -> p k j", j=3)
                bm = vec(l, "b_mod", 0, 6 * DC)
                S.op("dve", lambda: nc.vector.scalar_tensor_tensor(out=modl, in0=mg3[:, :, 0], scalar=msk[:, 8:9], in1=bm,
                                                                   op0=ALU.mult, op1=ALU.add), r=["mg", "msk", "vsb"], w=["mod"])
                S.op("dve", lambda: nc.vector.scalar_tensor_tensor(out=modl, in0=mg3[:, :, 1], scalar=msk[:, 9:10], in1=modl,
                                                                   op0=ALU.mult, op1=ALU.add), r=["mg", "msk", "mod"], w=["mod"])
                S.op("dve", lambda: nc.vector.tensor_tensor(out=modc, in0=mg3[:, :, 2], in1=bm, op=ALU.add),
                     r=["mg", "vsb", "mod"], w=["mod"])
                for (mod_, off) in ((modl, 0), (modc, DC)):
                    S.op("dve", lambda mod_=mod_, off=off: nc.vector.scalar_tensor_tensor(
                        out=Amix[:, off:off + DC], in0=mod_[:, DC:2 * DC], scalar=1.0, in1=vec(l, "norm_mix", 0, DC),
                        op0=ALU.add, op1=ALU.mult), r=["mod", "vsb"], w=["mod"])
                    S.op("dve", lambda mod_=mod_, off=off: nc.vector.scalar_tensor_tensor(
                        out=Affn[:, off:off + DC], in0=mod_[:, 4 * DC:5 * DC], scalar=1.0, in1=vec(l, "norm_ffn", 0, DC),
                        op0=ALU.add, op1=ALU.mult), r=["mod", "vsb"], w=["mod"])
                S.dma("sp", lv, lamv[:, l * 4 * g.DH:(l + 1) * 4 * g.DH], w=["lv"])
                for i in range(2):
                    S.op("dve", lambda i=i: nc.vector.tensor_tensor(out=lt, in0=lv[:, (2 * i) * g.DH:(2 * i + 1) * g.DH],
                                                                    in1=lv[:, (2 * i + 1) * g.DH:(2 * i + 2) * g.DH],
                                                                    op=ALU.mult), r=["lv"], w=["lt"])
                    S.op("dve", lambda i=i: nc.vector.reduce_sum(out=ls[:, i:i + 1], in_=lt, axis=AX.X), r=["lt"], w=["ls"])
                S.op("act", lambda: nc.scalar.activation(out=ls, in_=ls, func=AF.Exp), r=["ls"], w=["ls"])
                S.op("dve", lambda: nc.vector.tensor_tensor(out=lam[:, 0:1], in0=ls[:, 1:2], in1=ls[:, 0:1], op=ALU.subtract),
                     r=["ls"], w=["lam"])
                S.op("dve", lambda: nc.vector.tensor_scalar(out=lam[:, 0:1], in0=lam[:, 0:1], scalar1=-lam_init, scalar2=None,
                                                            op0=ALU.add), r=["lam"], w=["lam"])
                S.op("dve", lambda: nc.vector.tensor_scalar(out=sgv, in0=vec(l, "subln"), scalar1=1.0 - lam_init,
                                                            scalar2=None, op0=ALU.mult), r=["vsb"], w=["lam"])
            S.barrier()
            if stop == (l, 'P0'):
                raise _Stop()

            with ExitStack() as es:
                T = g.T1
                hb = [es.enter_context(nc.sbuf_tensor(un("hb%d" % i), [128, T], F32)).ap() for i in range(4)]
                sq = [es.enter_context(nc.sbuf_tensor(un("sq%d" % i), [128, T], F32)).ap() for i in range(2)]
                tmp = [es.enter_context(nc.sbuf_tensor(un("tmp%d" % i), [128, T], F32)).ap() for i in range(2)]
                rstd = es.enter_context(nc.sbuf_tensor(un("rstd"), [128, T], F32)).ap()
                xm = es.enter_context(nc.sbuf_tensor(un("xm"), [128, DC, T], BF16)).ap()
                wbufs = [es.enter_context(nc.sbuf_tensor(un("wb%d" % i), [128, WB_K, WB_N], BF16)).ap() for i in range(g.NWB)]
                cosb = es.enter_context(nc.sbuf_tensor(un("cosb"), [128, NTOK], F32)).ap()
                sinb = es.enter_context(nc.sbuf_tensor(un("sinb"), [128, NTOK], F32)).ap()
                e1 = [es.enter_context(nc.sbuf_tensor(un("e1_%d" % i), [128, T], F32)).ap() for i in range(2)]
                e2 = [es.enter_context(nc.sbuf_tensor(un("e2_%d" % i), [128, T], F32)).ap() for i in range(2)]
                e3 = [es.enter_context(nc.sbuf_tensor(un("e3_%d" % i), [128, T], F32)).ap() for i in range(2)]
                ob = [es.enter_context(nc.sbuf_tensor(un("ob%d" % i), [128, T], BF16)).ap() for i in range(2)]
                vtb = [es.enter_context(nc.sbuf_tensor(un("vtb%d" % i), [128, 512], BF16)).ap() for i in range(2)]
                uf = es.enter_context(nc.sbuf_tensor(un("uf"), [128, HC, T], BF16)).ap()
                ccb = es.enter_context(nc.sbuf_tensor(un("ccb"), [128, HC, T], F32)).ap()
                csb = es.enter_context(nc.sbuf_tensor(un("csb"), [128, g.FKC, 2 * g.FGW], BF16)).ap()
                S.dma("sp", cosb, ropec, w=["rope"])
                S.dma("sp", sinb, ropes, w=["rope"])
                S.dma("sp", csb, csmat.rearrange("(c p) n -> p c n", p=128), w=["csb"])
                W = wview("w_in", l)
                cnt = {"e": 0}

                for (t0, Tn, isctx) in tiles_lat + [tile_ctx]:
                    off = DC if isctx else 0
                    mod_ = modc if isctx else modl
                    norm_mod(h_src, t0, Tn, Amix[:, off:off + DC], mod_[:, 0:DC], xm, "xm", (hb, sq, rstd, tmp))
                    for (so, sn_) in chunks(DC, 8):
                        S.dma("pool", xmT[so * 128:(so + sn_) * 128, t0:t0 + Tn].rearrange("(c p) t -> p c t", p=128),
                              xm[:, so:so + sn_, :Tn], r=["xm"], w=["xmT"])
                    acts = [reg(xm[:, c, :Tn], "xm") for c in range(DC)]
                    kgs = kg_full(D, acts)

                    def epi_qk(j, col, cw, p, which):
                        i = cnt["e"] % 2
                        cnt["e"] += 1
                        a, b_, c_, o_ = e1[i][:, :Tn], e2[i][:, :Tn], e3[i][:, :Tn], ob[i][:, :Tn]
                        pn, pr = ps[6], ps[7]
                        gname = "q_norm" if which == 0 else "k_norm"
                        S.op("act", lambda: nc.scalar.activation(out=a, in_=p, func=AF.Square), r=[psk(p)], w=[("e1", i)])
                        S.op("pe", lambda: nc.tensor.matmul(pn[:, :Tn], lhsT=blk64, rhs=a, start=True, stop=True),
                             r=[("e1", i), "cm"], w=[psk(pn)])
                        rsq(b_, pn[:, :Tn], [psk(pn)], [("e2", i)])
                        S.op("dve", lambda: nc.vector.scalar_tensor_tensor(out=a, in0=p, scalar=vec(l, gname), in1=b_,
                                                                           op0=ALU.mult, op1=ALU.mult),
                             r=[psk(p), ("e2", i), "vsb"], w=[("e1", i)])
                        S.op("pe", lambda: nc.tensor.matmul(pr[:, :Tn], lhsT=Rm, rhs=a, start=True, stop=True),
                             r=[("e1", i), "cm"], w=[psk(pr)])
                        S.op("dve", lambda: nc.vector.tensor_tensor(out=b_, in0=a, in1=cosb[:, t0:t0 + Tn], op=ALU.mult),
                             r=[("e1", i), "rope"], w=[("e2", i)])
                        S.op("dve", lambda: nc.vector.tensor_tensor(out=c_, in0=pr[:, :Tn], in1=sinb[:, t0:t0 + Tn], op=ALU.mult),
                             r=[psk(pr), "rope"], w=[("e3", i)])
                        S.op("dve", lambda: nc.vector.tensor_tensor(out=o_, in0=b_, in1=c_, op=ALU.add),
                             r=[("e2", i), ("e3", i)], w=[("ob", i)])
                        dst = qT if which == 0 else kT
                        r0 = col - which * BW
                        S.dma("pool", dst[r0:r0 + 128, t0:t0 + Tn], o_, r=[("ob", i)], w=["qT" if which == 0 else "kT"])

                    def epi_copy(j, col, cw, p, dst, r0, key):
                        i = cnt["e"] % 2
                        cnt["e"] += 1
                        a = e1[i][:, :Tn]
                        S.op("act", lambda: nc.scalar.copy(out=a, in_=p), r=[psk(p)], w=[("e1", i)])
                        S.dma("pool", dst[r0:r0 + 128, t0:t0 + Tn], a, r=[("e1", i)], w=[key])

                    bank_sets = [ps[0:4], ps[4:8]]
                    bs = {"i": 0}

                    def nb():
                        b = bank_sets[bs["i"] % 2]
                        bs["i"] += 1
                        return b

                    for sec in range(8):
                        if sec == 2:
                            for (co, cw) in chunks(BW, 512):
                                def epi_v(j, to, tw_, p):
                                    i = cnt["e"] % 2
                                    cnt["e"] += 1
                                    S.op("act", lambda: nc.scalar.copy(out=vtb[i][:tw_, :cw], in_=p),
                                         r=[psk(p)], w=[("vtb", i)])
                                    S.dma("pool", vS[t0 + to:t0 + to + tw_, co:co + cw], vtb[i][:tw_, :cw],
                                          r=[("vtb", i)], w=["vS"])
                                gemm(W, sec * BW + co, cw, kgs, Tn, epi_v, wbufs, ps[0:4], tm=True)
                            continue
                        pw = 384 if sec in (0, 1) else 512
                        for (co, cw) in chunks(BW, pw):
                            col0 = sec * BW + co
                            if sec in (0, 1):
                                epi = lambda j, col, cw_, p, sec=sec: epi_qk(j, col, cw_, p, sec)
                                banks = ps[0:3] if bs["i"] % 2 == 0 else ps[3:6]
                                bs["i"] += 1
                            elif sec == 3:
                                epi = lambda j, col, cw_, p: epi_copy(j, col, cw_, p, upool, col - 3 * BW, "upool")
                                banks = nb()
                            elif sec == 4:
                                def epi(j, col, cw_, p):
                                    c = (col - 4 * BW) // 128
                                    S.op("act", lambda: nc.scalar.copy(out=uf[:, c, :Tn], in_=p),
                                         r=[psk(p)], w=[("uf", c)])
                                banks = nb()
                            elif sec == 5:
                                epi = lambda j, col, cw_, p: epi_copy(j, col, cw_, p, cbT, col - 5 * BW, "cbT")
                                banks = nb()
                            elif sec == 6:
                                def epi(j, col, cw_, p):
                                    c = (col - 6 * BW) // 128
                                    S.op("act", lambda: nc.scalar.copy(out=ccb[:, c, :Tn], in_=p),
                                         r=[psk(p)], w=[("ccb", c)])
                                banks = nb()
                            else:
                                def epi(j, col, cw_, p):
                                    c = (col - 7 * BW) // 128
                                    i = cnt["e"] % 2
                                    cnt["e"] += 1
                                    a = e1[i][:, :Tn]
                                    S.op("dve", lambda: nc.vector.tensor_tensor(out=a, in0=p, in1=ccb[:, c, :Tn], op=ALU.mult),
                                         r=[psk(p), ("ccb", c)], w=[("e1", i)])
                                    S.dma("pool", zT[c * 128:(c + 1) * 128, t0:t0 + Tn], a, r=[("e1", i)], w=["zT"])
                                banks = nb()
                            gemm(W, col0, cw, kgs, Tn, epi, wbufs, banks)
                        if sec == 4:
                            for gi in range(g.FG):
                                for si, (to, tw_) in enumerate(chunks(Tn, 128)):
                                    pb = ps[si % 4]
                                    for kc in range(g.FKC):
                                        c = gi * g.FKC + kc
                                        S.op("pe", lambda c=c, kc=kc, to=to, tw_=tw_, pb=pb: nc.tensor.matmul(
                                            pb[:tw_, :2 * g.FGW], lhsT=uf[:, c, to:to + tw_], rhs=csb[:, kc, :],
                                            start=(kc == 0), stop=(kc == g.FKC - 1)),
                                            r=[("uf", c), "csb"], w=[psk(pb)])
                                    i = cnt["e"] % 2
                                    cnt["e"] += 1
                                    S.op("act", lambda i=i, tw_=tw_, pb=pb: nc.scalar.copy(out=vtb[i][:tw_, :2 * g.FGW],
                                                                                          in_=pb[:tw_, :2 * g.FGW]),
                                         r=[psk(pb)], w=[("vtb", i)])
                                    S.dma("pool", gS[t0 + to:t0 + to + tw_, gi * 2 * g.FGW:(gi + 1) * 2 * g.FGW],
                                          vtb[i][:tw_, :2 * g.FGW], r=[("vtb", i)], w=["gS"])
                for bi, (srcT, key) in enumerate(((upool, "upool"), (zT, "zT"))):
                    for (ro, rn) in chunks(BW, 256):
                        rows = slice(bi * BW + ro, bi * BW + ro + rn)
                        S.dma("pool", slab[rows, 0:8], srcT[ro:ro + rn, 0:8], r=[key], w=["slab"])
                        S.dma("pool", slab[rows, 8:16], srcT[ro:ro + rn, NL - 8:NL], r=[key], w=["slab"])
                        S.dma("pool", slab[rows, 16:24], srcT[ro:ro + rn, NL:NL + 8], r=[key], w=["slab"])
                        S.dma("pool", slab[rows, 24:32], srcT[ro:ro + rn, NTOK - 8:NTOK], r=[key], w=["slab"])
                for h in range(g.H):
                    S.gather(GRP4, kT[h * 128:(h + 1) * 128, :], kG[h * 512:(h + 1) * 512, :], r=["kT"], w=[("kG", h)])
                for pi_, (p0, pm) in enumerate(vpcs):
                    S.gather(GRP4, vS[p0:p0 + pm, :], vG[4 * p0:4 * p0 + 4 * pm, :], r=["vS"], w=[("vG", pi_)])
                for pi_, (p0, pm) in enumerate(gpcs):
                    S.gather(GRP4, gS[p0:p0 + pm, :], gG[4 * p0:4 * p0 + 4 * pm, :], r=["gS"], w=[("gG", pi_)])
                S.gather(GRP4, slab, slabG, r=["slab"], w=["slabG"])
            S.barrier()
            if stop == (l, 'P1'):
                raise _Stop()

            seqs = [(0, NL, NLB, False)] + ([] if last else [(NL, NC_, NCB, True)])
            with ExitStack() as es:
                T = g.T1
                KB = [es.enter_context(nc.sbuf_tensor(un("KB%d" % i), [128, 4 * NTOK], BF16)).ap() for i in range(2)]
                nkc_max = 4 * len(chunks(NTOK, 128))
                VB = [es.enter_context(nc.sbuf_tensor(un("VB%d" % i), [128, nkc_max, 128], BF16)).ap() for i in range(2)]
                QB = [es.enter_context(nc.sbuf_tensor(un("QB%d" % i), [128, T], BF16)).ap() for i in range(2)]
                PB = [es.enter_context(nc.sbuf_tensor(un("PB%d" % i), [128, T], BF16)).ap() for i in range(4)]
                a1 = [es.enter_context(nc.sbuf_tensor(un("a1_%d" % i), [128, T], F32)).ap() for i in range(2)]
                a2 = es.enter_context(nc.sbuf_tensor(un("a2"), [128, T], F32)).ap()
                a3 = es.enter_context(nc.sbuf_tensor(un("a3"), [128, T], F32)).ap()
                ao = [es.enter_context(nc.sbuf_tensor(un("ao%d" % i), [128, T], BF16)).ap() for i in range(2)]
                scale = float(g.DH) ** -0.5
                hi = 0
                for (s0, sn, sN, isctx) in seqs:
                    if not isctx:
                        kcl = [(r, o, n) for r in range(4) for (o, n) in chunks(NTOK, 128)]
                    else:
                        kcl = [(r, NL + o, n) for r in range(4) for (o, n) in chunks(NC_, 128)]
                    for h in range(g.H):
                        kb, vb = KB[hi % 2], VB[hi % 2]
                        kk, vk = ("KB", hi % 2), ("VB", hi % 2)
                        hi += 1
                        for r in range(4):
                            S.dma("sp", kb[:, r * NTOK:(r + 1) * NTOK], kG[h * 512 + r * 128: h * 512 + (r + 1) * 128, :],
                                  r=["kG"], w=[kk])
                        for ci, (r, o, n) in enumerate(kcl):
                            vr = piece_row(vpcs, r, o)
                            S.dma("sp", vb[:n, ci, :], vG[vr: vr + n, h * 128:(h + 1) * 128],
                                  r=["vG"], w=[vk])
                        qi = 0
                        for (t0, Tn) in [(s0 + o, n) for (o, n) in chunks(sn, T)]:
                            qb = QB[qi % 2]
                            qk = ("QB", qi % 2)
                            qi += 1
                            S.dma("sp", qb[:, :Tn], qT[h * 128:(h + 1) * 128, t0:t0 + Tn], r=["qT"], w=[qk])
                            po = [ps[0], ps[1]]
                            psm = [ps[2], ps[3]]
                            pi = 0
                            for ci, (r, o, n) in enumerate(kcl):
                                for c in range(2):
                                    pS = ps[4 + pi % 4]
                                    pb = PB[pi % 4]
                                    pk = ("PB", pi % 4)
                                    pi += 1
                                    lo = c * 64
                                    S.op("pe", lambda pS=pS, lo=lo, r=r, o=o, n=n: nc.tensor.matmul(
                                        pS[:n, :Tn], lhsT=kb[lo:lo + 64, r * NTOK + o: r * NTOK + o + n],
                                        rhs=qb[lo:lo + 64, :Tn], start=True, stop=True),
                                        r=[kk, qk], w=[psk(pS)])
                                    S.op("act", lambda pS=pS, pb=pb, n=n: nc.scalar.activation(
                                        out=pb[:n, :Tn], in_=pS[:n, :Tn], func=AF.Exp, scale=scale),
                                        r=[psk(pS)], w=[pk])
                                    S.op("pe", lambda pb=pb, c=c, ci=ci, n=n: nc.tensor.matmul(
                                        po[c][:, :Tn], lhsT=vb[:n, ci, :], rhs=pb[:n, :Tn], start=(ci == 0),
                                        stop=(ci == len(kcl) - 1)), r=[vk, pk], w=[psk(po[c])])
                                    S.op("pe", lambda pb=pb, c=c, ci=ci, n=n: nc.tensor.matmul(
                                        psm[c][:, :Tn], lhsT=onesb[:n, :], rhs=pb[:n, :Tn], start=(ci == 0),
                                        stop=(ci == len(kcl) - 1)), r=["onesb", pk], w=[psk(psm[c])])
                            for c in range(2):
                                S.op("dve", lambda c=c: nc.vector.reciprocal(out=a2[:, :Tn], in_=psm[c][:, :Tn]),
                                     r=[psk(psm[c])], w=["a2"])
                                S.op("dve", lambda c=c: nc.vector.tensor_tensor(out=a1[c][:, :Tn], in0=po[c][:, :Tn],
                                                                                in1=a2[:, :Tn], op=ALU.mult),
                                     r=[psk(po[c]), "a2"], w=[("a1", c)])
                            S.op("dve", lambda: nc.vector.scalar_tensor_tensor(out=a3[:, :Tn], in0=a1[1][:, :Tn],
                                                                               scalar=lam[:, 0:1], in1=a1[0][:, :Tn],
                                                                               op0=ALU.mult, op1=ALU.add),
                                 r=[("a1", 0), ("a1", 1), "lam"], w=["a3"])
                            S.op("act", lambda: nc.scalar.activation(out=a2[:, :Tn], in_=a3[:, :Tn], func=AF.Square),
                                 r=["a3"], w=["a2"])
                            pn = ps[4]
                            S.op("pe", lambda: nc.tensor.matmul(pn[:, :Tn], lhsT=ones128, rhs=a2[:, :Tn], start=True, stop=True),
                                 r=["a2", "cm"], w=[psk(pn)])
                            rsq(a2[:, :Tn], pn[:, :Tn], [psk(pn)], ["a2"])
                            oo = ao[qi % 2]
                            S.op("dve", lambda oo=oo: nc.vector.scalar_tensor_tensor(out=oo[:, :Tn], in0=a3[:, :Tn], scalar=sgv[:, 0:1],
                                                                                     in1=a2[:, :Tn], op0=ALU.mult, op1=ALU.mult),
                                 r=["a3", "a2", "lam"], w=[("ao", qi % 2)])
                            S.dma("pool", yT[h * 128:(h + 1) * 128, t0:t0 + Tn], oo[:, :Tn], r=[("ao", qi % 2)], w=["yT"])
            S.barrier()
            if stop == (l, 'ATT'):
                raise _Stop()

            with ExitStack() as es:
                T = g.T1
                GB = [es.enter_context(nc.sbuf_tensor(un("GB%d" % i), [128, 2 * BW], BF16)).ap() for i in range(3)]
                TW = [es.enter_context(nc.sbuf_tensor(un("TW%d" % i), [128, 2, T], BF16)).ap() for i in range(3)]
                fo = [es.enter_context(nc.sbuf_tensor(un("fo%d" % i), [128, T], BF16)).ap() for i in range(2)]
                for (s0, sn, sN, isctx) in seqs:
                    tw = twC if isctx else twL
                    if not isctx:
                        ncl = [(r, o, 128, r * (NL // 128) + o // 128) for r in range(4) for (o, _) in chunks(NL, 128)]
                        nch = NLB // 128
                    else:
                        ncl = [(r, NL, NC_, r) for r in range(4)]
                        nch = 4
                    for (k0, kn) in chunks(sn, T):
                        li = 0
                        for idx, (r, o, n, a) in enumerate(ncl):
                            gb, twb = GB[li % 3], TW[li % 3]
                            gk, tk = ("GB", li % 3), ("TW", li % 3)
                            li += 1
                            gr = piece_row(gpcs, r, o)
                            S.dma("sp", gb[:n, :], gG[gr: gr + n, :], r=["gG"], w=[gk])
                            for ci in range(2):
                                r0 = ci * nch * 128 + a * 128
                                S.dma("sp", twb[:n, ci, :kn], tw[r0:r0 + n, k0:k0 + kn], r=["tw"], w=[tk])
                            for oc in range(HC):
                                gi, w_ = oc // g.FKC, oc % g.FKC
                                for ci in range(2):
                                    c0 = gi * 2 * g.FGW + ci * g.FGW + w_ * 128
                                    S.op("pe", lambda oc=oc, ci=ci, c0=c0, gb=gb, twb=twb, n=n, idx=idx: nc.tensor.matmul(
                                        ps[oc][:, :kn], lhsT=gb[:n, c0:c0 + 128], rhs=twb[:n, ci, :kn],
                                        start=(idx == 0 and ci == 0), stop=(idx == len(ncl) - 1 and ci == 1)),
                                        r=[gk, tk], w=[psk(ps[oc])])
                        for oc in range(HC):
                            f_ = fo[oc % 2]
                            S.op("act", lambda oc=oc, f_=f_: nc.scalar.activation(out=f_[:, :kn], in_=ps[oc][:, :kn], func=AF.Identity,
                                                                                  scale=float(sN) ** -0.5),
                                 r=[psk(ps[oc])], w=[("fo", oc % 2)])
                            S.dma("pool", yT[2 * BW + oc * 128: 2 * BW + (oc + 1) * 128, s0 + k0: s0 + k0 + kn], f_[:, :kn],
                                  r=[("fo", oc % 2)], w=["yT"])
            S.barrier()
            if stop == (l, 'FOUR'):
                raise _Stop()

            with ExitStack() as es:
                NB = NL + 16
                ub = [es.enter_context(nc.sbuf_tensor(un("ub%d" % i), [128, NB], F32)).ap() for i in range(3)]
                ic = es.enter_context(nc.sbuf_tensor(un("ic"), [128, NL], F32)).ap()
                pl = es.enter_context(nc.sbuf_tensor(un("pl"), [128, g.PKC, NL], BF16)).ap()
                sl = es.enter_context(nc.sbuf_tensor(un("sl"), [128, 4, 32], F32)).ap()
                pwb = es.enter_context(nc.sbuf_tensor(un("pwb"), [128, g.PKC, g.PGW], BF16)).ap()
                cb = es.enter_context(nc.sbuf_tensor(un("cbb"), [128, NL], F32)).ap()
                po_ = [es.enter_context(nc.sbuf_tensor(un("po%d" % i), [128, 512], BF16)).ap() for i in range(2)]
                cvo = es.enter_context(nc.sbuf_tensor(un("cvo"), [128, NL], BF16)).ap()
                PW = wview("pool_w", l)

                def load_halo(buf, srcT, c, s0, sn, slab_row0, isctx, hw):
                    S.dma("sp", buf[:, hw:hw + sn], srcT[c * 128:(c + 1) * 128, s0:s0 + sn], r=["upool", "zT"], w=[("ub", id(buf))])
                    S.dma("sp", sl, slabG.rearrange("(r q) c -> q r c", r=4)[slab_row0 + c * 128: slab_row0 + (c + 1) * 128, :, :],
                          r=["slabG"], w=["sl"])
                    cL = 24 if isctx else 8
                    cR = 16 if isctx else 0
                    for side, (dst0, c0, mo) in enumerate(((0, cL + 8 - hw, 0), (hw + sn, cR, 4))):
                        d = buf[:, dst0:dst0 + hw]
                        S.op("dve", lambda d=d, c0=c0, mo=mo: nc.vector.tensor_scalar(
                            out=d, in0=sl[:, 0, c0:c0 + hw], scalar1=msk[:, mo:mo + 1], scalar2=None, op0=ALU.mult),
                            r=["sl", "msk"], w=[("ub", id(buf))])
                        for r in range(1, 4):
                            S.op("dve", lambda d=d, c0=c0, mo=mo, r=r: nc.vector.scalar_tensor_tensor(
                                out=d, in0=sl[:, r, c0:c0 + hw], scalar=msk[:, mo + r:mo + r + 1], in1=d,
                                op0=ALU.mult, op1=ALU.add), r=["sl", "msk", ("ub", id(buf))], w=[("ub", id(buf))])

                for (s0, sn, sN, isctx) in seqs:
                    for gi in range(g.PG):
                        w_ = g.POOLW[gi]
                        S.dma("sp", pwb, PW[gi * g.PGW:(gi + 1) * g.PGW, :].rearrange("(c p) n -> p c n", p=128),
                              r=[("wf", "pool_w", l)], w=["pwb"])
                        S.dma("sp", ic[:, :sn], invcnt[gi:gi + 1, s0:s0 + sn].partition_broadcast(128), w=["ic"])
                        for kc in range(g.PKC):
                            c = gi * g.PKC + kc
                            u0, u1, u2 = ub
                            load_halo(u0, upool, c, s0, sn, 0, isctx, 8)
                            cur, nxt_ = u0, u1
                            step = 1
                            ln = sn + 16
                            while step < w_:
                                ln2 = ln - step
                                S.op("pool", lambda cur=cur, nxt_=nxt_, step=step, ln2=ln2: nc.gpsimd.tensor_tensor(
                                    out=nxt_[:, :ln2], in0=cur[:, :ln2], in1=cur[:, step:step + ln2], op=ALU.add),
                                    r=[("ub", id(cur))], w=[("ub", id(nxt_))])
                                ln = ln2
                                cur, nxt_ = nxt_, (u2 if nxt_ is u1 else u1)
                                step *= 2
                            st = 8 - w_ // 2
                            other = u2 if cur is u1 else u1
                            S.op("dve", lambda cur=cur, other=other, st=st: nc.vector.tensor_tensor(
                                out=other[:, :sn], in0=cur[:, st:st + sn], in1=ic[:, :sn], op=ALU.mult),
                                r=[("ub", id(cur)), "ic"], w=[("ub", id(other))])
                            S.op("dve", lambda other=other, kc=kc: nc.vector.tensor_tensor(
                                out=pl[:, kc, :sn], in0=other[:, :sn], in1=u0[:, 8:8 + sn], op=ALU.subtract),
                                r=[("ub", id(other)), ("ub", id(u0))], w=[("pl", kc)])
                        for oc in range(g.PKC):
                            co = gi * g.PKC + oc
                            for ti, (to, tn) in enumerate(chunks(sn, 512)):
                                pb = ps[(ti + oc) % 8]
                                for kc in range(g.PKC):
                                    S.op("pe", lambda kc=kc, oc=oc, to=to, tn=tn, pb=pb: nc.tensor.matmul(
                                        pb[:, :tn], lhsT=pwb[:, kc, oc * 128:(oc + 1) * 128], rhs=pl[:, kc, to:to + tn],
                                        start=(kc == 0), stop=(kc == g.PKC - 1)), r=["pwb", ("pl", kc)], w=[psk(pb)])
                                o_ = po_[ti % 2]
                                S.op("act", lambda pb=pb, o_=o_, tn=tn, co=co: nc.scalar.activation(
                                    out=o_[:, :tn], in_=pb[:, :tn], func=AF.Identity, scale=vec(l, "pool_scale", co)),
                                    r=[psk(pb), "vsb"], w=[("po", ti % 2)])
                                S.dma("pool", yT[BW + co * 128: BW + (co + 1) * 128, s0 + to:s0 + to + tn], o_[:, :tn],
                                      r=[("po", ti % 2)], w=["yT"])
                    for c in range(HC):
                        u0, u1, u2 = ub
                        load_halo(u0, zT, c, s0, sn, BW, isctx, 1)
                        S.dma("sp", cb[:, :sn], cbT[c * 128:(c + 1) * 128, s0:s0 + sn], r=["cbT"], w=["cb"])
                        S.op("dve", lambda c=c: nc.vector.tensor_scalar(out=u1[:, :sn], in0=u0[:, 0:sn], scalar1=vec(l, "conv_w", c),
                                                                        scalar2=None, op0=ALU.mult),
                             r=[("ub", id(u0)), "vsb"], w=[("ub", id(u1))])
                        for j in (1, 2):
                            S.op("dve", lambda c=c, j=j: nc.vector.scalar_tensor_tensor(
                                out=u1[:, :sn], in0=u0[:, j:j + sn], scalar=vec(l, "conv_w", j * HC + c), in1=u1[:, :sn],
                                op0=ALU.mult, op1=ALU.add), r=[("ub", id(u0)), ("ub", id(u1)), "vsb"], w=[("ub", id(u1))])
                        S.op("pool", lambda: nc.gpsimd.tensor_tensor(out=cvo[:, :sn], in0=u1[:, :sn], in1=cb[:, :sn], op=ALU.mult),
                             r=[("ub", id(u1)), "cb"], w=["cvo"])
                        S.dma("pool", yT[3 * BW + c * 128: 3 * BW + (c + 1) * 128, s0:s0 + sn], cvo[:, :sn], r=["cvo"], w=["yT"])
            S.barrier()
            if stop == (l, 'POOL'):
                raise _Stop()

            with ExitStack() as es:
                T = g.T3
                NHC = ((g.NE // 2) * len(chunks(g.DFE, 128))) if moe else len(chunks(g.DFF, 128))
                hb = [es.enter_context(nc.sbuf_tensor(un("hb%d" % i), [128, T], F32)).ap() for i in range(4)]
                sq = [es.enter_context(nc.sbuf_tensor(un("sq%d" % i), [128, T], F32)).ap() for i in range(2)]
                tmp = [es.enter_context(nc.sbuf_tensor(un("tmp%d" % i), [128, T], F32)).ap() for i in range(2)]
                zfb = [es.enter_context(nc.sbuf_tensor(un("zf%d" % i), [128, T], F32)).ap() for i in range(2)]
                rstd = es.enter_context(nc.sbuf_tensor(un("rstd"), [128, T], F32)).ap()
                xm = es.enter_context(nc.sbuf_tensor(un("xm3"), [128, DC, T], BF16)).ap()
                yb = es.enter_context(nc.sbuf_tensor(un("yb"), [128, 4 * HC, T], BF16)).ap()
                mer = es.enter_context(nc.sbuf_tensor(un("mer"), [128, DC, T], BF16)).ap()
                hid = es.enter_context(nc.sbuf_tensor(un("hid"), [128, NHC, T], BF16)).ap()
                wbufs = [es.enter_context(nc.sbuf_tensor(un("wb%d" % i), [128, WB_K, WB_N], BF16)).ap() for i in range(g.NWB)]
                sg = [es.enter_context(nc.sbuf_tensor(un("sg%d" % i), [128, T], F32)).ap() for i in range(4)]
                acc = [es.enter_context(nc.sbuf_tensor(un("acc%d" % i), [128, T], F32)).ap() for i in range(4)]
                hn = [es.enter_context(nc.sbuf_tensor(un("hn%d" % i), [128, T], F32)).ap() for i in range(2)]
                bcb = es.enter_context(nc.sbuf_tensor(un("bcb"), [128, g.NE, T], F32)).ap()
                lg = es.enter_context(nc.sbuf_tensor(un("lg"), [128, 8 * g.NE], F32)).ap()
                cT = es.enter_context(nc.sbuf_tensor(un("cT"), [128, T], F32)).ap()
                WG, WBr, WO = wview("w_gate", l), wview("w_branch", l), wview("w_out", l)
                p3tiles = [(o, n, False) for (o, n) in chunks(NL, T)] + ([] if last else [(NL, NC_, True)])
                ctr = {"h": 0}
                for (t0, Tn, isctx) in p3tiles:
                    off = DC if isctx else 0
                    mod_ = modc if isctx else modl
                    for (so, sn_) in chunks(DC, 8):
                        S.dma("sp", xm[:, so:so + sn_, :Tn],
                              xmT[so * 128:(so + sn_) * 128, t0:t0 + Tn].rearrange("(c p) t -> p c t", p=128), r=["xmT"], w=["xm"])
                    for (so, sn_) in chunks(4 * HC, 8):
                        S.dma("sp", yb[:, so:so + sn_, :Tn],
                              yT[so * 128:(so + sn_) * 128, t0:t0 + Tn].rearrange("(c p) t -> p c t", p=128), r=["yT"], w=["yb"])
                    xacts = [reg(xm[:, c, :Tn], "xm") for c in range(DC)]
                    kgx = kg_full(D, xacts)
                    for (po, pw) in chunks(D, 512):
                        npc = len(chunks(pw, 128))
                        for bi in range(4):
                            def epi_g(j, col, cw, p, bi=bi):
                                S.op("act", lambda: nc.scalar.activation(out=sg[j][:, :Tn], in_=p, func=AF.Sigmoid),
                                     r=[psk(p)], w=[("sg", j)])
                            gemm(WG, bi * D + po, pw, kgx, Tn, epi_g, wbufs, ps[0:4])
                            yacts = [reg(yb[:, bi * HC + c, :Tn], "yb") for c in range(HC)]
                            kgy = [[(bi * BW + o_, n_, yacts[ci]) for ci, (o_, n_) in enumerate(chunks(BW, 128))]]
                            kgy = [kgy[0][i:i + 16] for i in range(0, len(kgy[0]), 16)]

                            def epi_b(j, col, cw, p, bi=bi, po=po):
                                oc = (po // 128) + j
                                if bi == 0:
                                    S.op("dve", lambda: nc.vector.tensor_tensor(out=acc[j][:, :Tn], in0=p, in1=sg[j][:, :Tn], op=ALU.mult),
                                         r=[psk(p), ("sg", j)], w=[("acc", j)])
                                else:
                                    S.op("dve", lambda: nc.vector.tensor_tensor(out=sg[j][:, :Tn], in0=p, in1=sg[j][:, :Tn], op=ALU.mult),
                                         r=[psk(p), ("sg", j)], w=[("sg", j)])
                                    if bi < 3:
                                        S.op("pool", lambda: nc.gpsimd.tensor_tensor(out=acc[j][:, :Tn], in0=acc[j][:, :Tn],
                                                                                     in1=sg[j][:, :Tn], op=ALU.add),
                                             r=[("acc", j), ("sg", j)], w=[("acc", j)])
                                    else:
                                        S.op("pool", lambda: nc.gpsimd.tensor_tensor(out=mer[:, oc, :Tn], in0=acc[j][:, :Tn],
                                                                                     in1=sg[j][:, :Tn], op=ALU.add),
                                             r=[("acc", j), ("sg", j)], w=["mer"])
                            gemm(WBr, po, pw, kgy, Tn, epi_b, wbufs, ps[4:8])
                    macts = [reg(mer[:, c, :Tn], "mer") for c in range(DC)]
                    kgm = kg_full(D, macts)

                    def epi_res(j, col, cw, p, src, dst, gcol, dkey):
                        oc = col // 128
                        i = ctr["h"] % 2
                        ctr["h"] += 1
                        hb_ = hb[i]
                        S.dma("sp", hb_[:, :Tn], src[oc * 128:(oc + 1) * 128, t0:t0 + Tn], r=[("h", src.tensor.name)], w=[("hb", i)])
                        S.op("dve", lambda: nc.vector.scalar_tensor_tensor(out=hn[i][:, :Tn], in0=p, scalar=mod_[:, gcol * DC + oc: gcol * DC + oc + 1],
                                                                           in1=hb_[:, :Tn], op0=ALU.mult, op1=ALU.add),
                             r=[psk(p), ("hb", i), "mod"], w=[("hn", i)])
                        if dst is outT:
                            S.dma("pool", dst[oc * 128:(oc + 1) * 128, t0:t0 + Tn], hn[i][:, :Tn], r=[("hn", i)], w=[dkey])
                        else:
                            S.dma("pool", dst[oc * 128:(oc + 1) * 128, t0:t0 + Tn], hn[i][:, :Tn], r=[("hn", i)], w=[dkey])

                    bsel = 0
                    for (po, pw) in chunks(D, 512):
                        banks = ps[0:4] if bsel % 2 == 0 else ps[4:8]
                        bsel += 1
                        gemm(WO, po, pw, kgm, Tn, lambda j, col, cw, p: epi_res(j, col, cw, p, h_src, hmid, 2, ("h", hmid.tensor.name)),
                             wbufs, banks)
                    router = {"zf": zfb} if moe else None
                    norm_mod(hmid, t0, Tn, Affn[:, off:off + DC], mod_[:, 3 * DC:4 * DC], xm, "xm", (hb, sq, rstd, tmp), router=router)
                    zacts = [reg(xm[:, c, :Tn], "xm") for c in range(DC)]
                    kgz = kg_full(D, zacts)
                    if moe:
                        for si, (to, tw_) in enumerate(chunks(Tn, 128)):
                            L_ = lg[:tw_, 0:g.NE]
                            E1 = lg[:tw_, g.NE:2 * g.NE]
                            L2 = lg[:tw_, 2 * g.NE:3 * g.NE]
                            E2 = lg[:tw_, 3 * g.NE:4 * g.NE]
                            m1 = lg[:tw_, 4 * g.NE:4 * g.NE + 1]
                            m2 = lg[:tw_, 4 * g.NE + 1:4 * g.NE + 2]
                            w1 = lg[:tw_, 4 * g.NE + 2:4 * g.NE + 3]
                            w2 = lg[:tw_, 4 * g.NE + 3:4 * g.NE + 4]
                            CB_ = lg[:tw_, 5 * g.NE:6 * g.NE]
                            pl_ = ps[5 + si]
                            V = nc.vector
                            S.op("dve", lambda: V.tensor_copy(out=L_, in_=pl_[:tw_, :g.NE]), r=[psk(pl_)], w=["lg"])
                            S.op("dve", lambda: V.reduce_max(out=m1, in_=L_, axis=AX.X), r=["lg"], w=["lg"])
                            S.op("dve", lambda: V.tensor_scalar(out=E1, in0=L_, scalar1=m1, scalar2=None, op0=ALU.is_equal), r=["lg"], w=["lg"])
                            S.op("dve", lambda: V.scalar_tensor_tensor(out=L2, in0=E1, scalar=-1e30, in1=L_, op0=ALU.mult, op1=ALU.add),
                                 r=["lg"], w=["lg"])
                            S.op("dve", lambda: V.reduce_max(out=m2, in_=L2, axis=AX.X), r=["lg"], w=["lg"])
                            S.op("dve", lambda: V.tensor_scalar(out=E2, in0=L2, scalar1=m2, scalar2=None, op0=ALU.is_equal), r=["lg"], w=["lg"])
                            S.op("dve", lambda: V.tensor_tensor(out=w2, in0=m2, in1=m1, op=ALU.subtract), r=["lg"], w=["lg"])
                            S.op("act", lambda: nc.scalar.activation(out=w2, in_=w2, func=AF.Exp), r=["lg"], w=["lg"])
                            S.op("dve", lambda: V.tensor_scalar(out=w1, in0=w2, scalar1=1.0, scalar2=None, op0=ALU.add), r=["lg"], w=["lg"])
                            S.op("dve", lambda: V.reciprocal(out=w1, in_=w1), r=["lg"], w=["lg"])
                            S.op("dve", lambda: V.tensor_tensor(out=w2, in0=w2, in1=w1, op=ALU.mult), r=["lg"], w=["lg"])
                            S.op("dve", lambda: V.tensor_scalar(out=CB_, in0=E1, scalar1=w1, scalar2=None, op0=ALU.mult), r=["lg"], w=["lg"])
                            S.op("dve", lambda: V.scalar_tensor_tensor(out=CB_, in0=E2, scalar=w2, in1=CB_, op0=ALU.mult, op1=ALU.add),
                                 r=["lg"], w=["lg"])
                            pt = ps[7]
                            S.op("pe", lambda: nc.tensor.transpose(pt[:g.NE, :tw_], CB_, ident[:tw_, :tw_]), r=["lg", "cm"], w=[psk(pt)])
                            S.op("dve", lambda: V.tensor_copy(out=cT[:g.NE, to:to + tw_], in_=pt[:g.NE, :tw_]), r=[psk(pt)], w=["cT"])
                        for e in range(g.NE):
                            pb = ps[e % 4]
                            S.op("pe", lambda e=e, pb=pb: nc.tensor.matmul(pb[:, :Tn], lhsT=esel[:g.NE, e, :], rhs=cT[:g.NE, :Tn],
                                                                           start=True, stop=True), r=["cT", "esel"], w=[psk(pb)])
                            S.op("act", lambda e=e, pb=pb: nc.scalar.copy(out=bcb[:, e, :Tn], in_=pb[:, :Tn]),
                                 r=[psk(pb)], w=[("bcb", e)])
                    egroups = [list(range(0, g.NE // 2)), list(range(g.NE // 2, g.NE))] if moe else [[None]]
                    for gi_, grp in enumerate(egroups):
                        hci = 0
                        hrows = []
                        for e in grp:
                            if moe:
                                W1, W3, krow0, NF = wview("moe_w1", 0), wview("moe_w3", 0), e * D, g.DFE
                            else:
                                W1, W3, krow0, NF = wview("ffn_w1", 0), wview("ffn_w3", 0), 0, g.DFF
                            kge = [[(krow0 + r0, nr, a) for (r0, nr, a) in kg] for kg in kgz]
                            for (po, pw) in chunks(NF, 512):
                                def epi1(j, col, cw, p):
                                    S.op("act", lambda: nc.scalar.activation(out=sg[j][:cw, :Tn], in_=p, func=AF.Silu),
                                         r=[psk(p)], w=[("sg", j)])
                                gemm(W1, po, pw, kge, Tn, epi1, wbufs, ps[0:4])
                                base = hci

                                def epi3(j, col, cw, p, base=base, e=e):
                                    if e is None:
                                        S.op("dve", lambda: nc.vector.tensor_tensor(out=hid[:cw, base + j, :Tn], in0=p, in1=sg[j][:cw, :Tn], op=ALU.mult),
                                             r=[psk(p), ("sg", j)], w=[("hid", base + j)])
                                    else:
                                        S.op("dve", lambda: nc.vector.tensor_tensor(out=sg[j][:cw, :Tn], in0=p, in1=sg[j][:cw, :Tn], op=ALU.mult),
                                             r=[psk(p), ("sg", j)], w=[("sg", j)])
                                        S.op("pool", lambda: nc.gpsimd.tensor_tensor(out=hid[:cw, base + j, :Tn], in0=sg[j][:cw, :Tn],
                                                                                     in1=bcb[:cw, e, :Tn], op=ALU.mult),
                                             r=[("sg", j), ("bcb", e)], w=[("hid", base + j)])
                                gemm(W3, po, pw, kge, Tn, epi3, wbufs, ps[4:8])
                                for j, (co, cw) in enumerate(chunks(pw, 128)):
                                    row0 = (e * g.DFE if e is not None else 0) + po + co
                                    hrows.append((row0, cw, reg(hid[:cw, hci, :Tn], ("hid", hci))))
                                    hci += 1
                        per = len(chunks(g.DFE, 128)) if moe else 22
                        kgh = [hrows[i:i + per] for i in range(0, len(hrows), per)]
                        W2 = wview("moe_w2", 0) if moe else wview("ffn_w2", 0)
                        fsrc = hmid if gi_ == 0 else hmid2
                        lastg = (gi_ == len(egroups) - 1)
                        fdst = h_dst if lastg else hmid2
                        fkey = ("out" if last else ("h", hA.tensor.name)) if lastg else ("h", hmid2.tensor.name)
                        for (po, pw) in chunks(D, 512):
                            banks = ps[0:4] if bsel % 2 == 0 else ps[4:8]
                            bsel += 1
                            gemm(W2, po, pw, kgh, Tn, lambda j, col, cw, p: epi_res(j, col, cw, p, fsrc, fdst, 5, fkey),
                                 wbufs, banks)
            S.barrier()
            if stop == (l, 'P3'):
                raise _Stop()
    except _Stop:
        S.barrier()

    S.final_wait("pool")
    return nc, S


esel = None


def _build(cfg):
    return build_program(cfg)


def host_tables(g, core):
    b, r = core // g.CPB, core % g.CPB
    NL, NC_, NTOK = g.NL, g.NC, g.NTOK
    pos = r * NL + np.arange(NL)
    row = (pos // g.GRID_W).astype(np.float32)
    col = (pos % g.GRID_W).astype(np.float32)
    freqs = (10000.0 ** (-np.arange(g.RF, dtype=np.float32) / g.RF)).astype(np.float32)
    p = np.arange(128)
    f = p % g.RF
    axis = (p % g.DH) // (2 * g.RF)
    ang = np.where(axis[:, None] == 0, row[None, :], col[None, :]).astype(np.float32) * freqs[f][:, None]
    ang = ang.astype(np.float32)
    cos = np.ones((128, NTOK), np.float32)
    sin = np.zeros((128, NTOK), np.float32)
    cos[:, :NL] = np.cos(ang)
    sin[:, :NL] = np.sin(ang)
    kpos = np.concatenate([r * NL + np.arange(NL), r * NC_ + np.arange(NC_)]).astype(np.float32)[None, :]
    inv = np.ones((g.PG, NTOK), np.float32)
    for gi, w in enumerate(g.POOLW):
        for (s0, n, tot, base) in ((0, NL, NL * g.CPB, r * NL), (NL, NC_, NC_ * g.CPB, r * NC_)):
            t = base + np.arange(n)
            lo = np.clip(t - w // 2, 0, tot - 1)
            hi = np.clip(t + w - 1 - w // 2, 0, tot - 1)
            inv[gi, s0:s0 + n] = 1.0 / (hi - lo + 1)
    misc = np.zeros((128, 16), np.float32)
    if r > 0:
        misc[:, r - 1] = 1.0
    if r < g.CPB - 1:
        misc[:, 4 + r + 1] = 1.0
    misc[:, 8 + b] = 1.0
    return cos, sin, kpos, inv, misc


def host_consts(g):
    ident = np.eye(128, dtype=np.float32)
    R = np.zeros((128, 128), np.float32)
    for m in range(128):
        q = m % g.DH
        if (q % (2 * g.RF)) < g.RF:
            R[m + g.RF, m] = -1.0
        else:
            R[m - g.RF, m] = 1.0
    onesD = np.full((128, 128), 1.0 / g.D, np.float32)
    blk = np.zeros((128, 128), np.float32)
    for i in range(128 // g.DH):
        blk[i * g.DH:(i + 1) * g.DH, i * g.DH:(i + 1) * g.DH] = 1.0 / g.DH
    ones128 = np.full((128, 128), 1.0 / 128, np.float32)
    spare = np.zeros((128, 128), np.float32)
    cm = np.concatenate([ident, R, onesD, blk, ones128, spare], axis=1)
    c = np.arange(g.FGW)
    ang = 2.0 * np.pi * np.outer(c, c) / g.FGW
    s = g.FGW ** -0.5
    cs = np.concatenate([-np.cos(ang) * s, np.sin(ang) * s], axis=1).astype(np.float32)
    return cm, cs.astype(ml_dtypes.bfloat16)


def prepare_inputs(g, inp):
    L = g.DEPTH
    D, DC = g.D, g.DC
    f32 = lambda a: np.ascontiguousarray(np.asarray(a, dtype=np.float32))
    x, ctx = f32(inp["x"]), f32(inp["ctx"])
    cond = np.stack([f32(inp["c"])[0], f32(inp["c"])[1], f32(inp["c_ctx"])], axis=0)
    condT = np.ascontiguousarray(cond.reshape(3, DC, 128).transpose(2, 1, 0)).reshape(128, DC * 3)
    vecs = np.zeros((128, L * g.NV), np.float32)

    def colmajor(v):
        return np.asarray(v, np.float32).reshape(-1, 128).T

    for l in range(L):
        def put(name, arr):
            o, n = g.V[name]
            vecs[:, l * g.NV + o: l * g.NV + o + n] = arr
        put("norm_mix", colmajor(inp["norm_mix"][l]))
        put("norm_ffn", colmajor(inp["norm_ffn"][l]))
        put("b_mod", colmajor(inp["b_mod"][l]))
        put("q_norm", np.tile(np.asarray(inp["q_norm"][l], np.float32), 128 // g.DH)[:, None])
        put("k_norm", np.tile(np.asarray(inp["k_norm"][l], np.float32), 128 // g.DH)[:, None])
        put("subln", np.asarray(inp["subln"][l], np.float32)[:, None])
        put("pool_scale", colmajor(inp["pool_scale"][l]))
        cw = np.asarray(inp["conv_w"][l], np.float32)
        put("conv_w", np.concatenate([colmajor(cw[j]) for j in range(3)], axis=1))
    lamv = np.zeros((128, L * 4 * g.DH), np.float32)
    for l in range(L):
        for i, nm in enumerate(("lambda_q1", "lambda_k1", "lambda_q2", "lambda_k2")):
            lamv[:, (l * 4 + i) * g.DH:(l * 4 + i + 1) * g.DH] = np.asarray(inp[nm][l], np.float32)[None, :]
    router = np.asarray(inp["router"][0], np.float32)
    routerT = np.ascontiguousarray(router.reshape(DC, 128, g.NE).transpose(1, 0, 2)).reshape(128, DC * g.NE)
    cm, cs = host_consts(g)
    esel_np = np.zeros((128, g.NE * 128), np.float32)
    for e in range(g.NE):
        esel_np[e, e * 128:(e + 1) * 128] = 1.0

    wfl = {
        "w_in": lambda l: inp["w_in"][l], "w_gate": lambda l: inp["w_gate"][l],
        "w_branch": lambda l: np.asarray(inp["w_branch"][l]).reshape(4 * g.BW, D),
        "w_out": lambda l: inp["w_out"][l],
        "pool_w": lambda l: np.asarray(inp["pool_w"][l]).reshape(g.PG * g.PGW, g.PGW),
        "ffn_w1": lambda l: inp["ffn_w1"][0], "ffn_w3": lambda l: inp["ffn_w3"][0], "ffn_w2": lambda l: inp["ffn_w2"][0],
        "moe_w1": lambda l: np.asarray(inp["moe_w1"][0]).reshape(g.NE * D, g.DFE),
        "moe_w3": lambda l: np.asarray(inp["moe_w3"][0]).reshape(g.NE * D, g.DFE),
        "moe_w2": lambda l: np.asarray(inp["moe_w2"][0]).reshape(g.NE * g.DFE, D),
    }
    nls = {"w_in": L, "w_gate": L, "w_branch": L, "w_out": L, "pool_w": L}
    wmod_full = np.asarray(inp["w_mod"], np.float32)
    maps = []
    for core in range(g.NCORES):
        b, r = core // g.CPB, core % g.CPB
        cos, sin, kpos, inv, misc = host_tables(g, core)
        xt = np.concatenate([x[b, r * g.NL:(r + 1) * g.NL, :].T, ctx[b, r * g.NC:(r + 1) * g.NC, :].T], axis=1)
        m = {
            "xT": np.ascontiguousarray(xt), "condT": condT,
            "wmod": np.ascontiguousarray(wmod_full[:, :, core * g.MODC:(core + 1) * g.MODC]),
            "vecs": vecs, "lamv": lamv, "routerT": routerT, "ropec": cos, "ropes": sin, "kpos": kpos,
            "invcnt": inv, "misc": misc, "cmat": cm, "csmat": cs, "eselm": esel_np,
        }
        for name, fn in wfl.items():
            for l in range(nls.get(name, 1)):
                Wm = np.asarray(fn(l), np.float32)
                K = Wm.shape[0]
                M_ = piece_rows(K, Wm.shape[1])
                sh = Wm.reshape(K // M_, 8, M_ // 8, Wm.shape[1])[:, core].reshape(K // 8, Wm.shape[1])
                m["%s_%d" % (name, l)] = np.ascontiguousarray(sh).reshape(128, -1)
        maps.append(m)
    return maps


_CACHE = {}


def run_cfg(g, inputs):
    key = (g.D, g.SEQ, g.CTX)
    if key not in _CACHE:
        _CACHE[key] = build_program(g)[0]
    nc = _CACHE[key]
    maps = prepare_inputs(g, inputs)
    res = run_bass_kernel_spmd(nc, maps, core_ids=list(range(g.NCORES)))
    out = np.zeros((g.B, g.SEQ, g.D), np.float32)
    for core in range(g.NCORES):
        b, r = core // g.CPB, core % g.CPB
        out[b, r * g.NL:(r + 1) * g.NL, :] = np.asarray(res.results[core]["outT"]).T
    return out


def kernel(**inputs):
    return run_cfg(FULL, inputs)
```
